# Optimizing a Trainium2 kernel written in Bass

```python
import jax, jax.numpy as jnp
from jax import lax
import numpy as np

D_MODEL = 2048
BATCH = 16
SEQ = 256
DEPTH = 4
DEC_BATCH = 2
DEC_SEQ = 1024
PAST_LEN = 256

GRID_W = 64
HEAD_DIM = 128
N_Q_A = 8
N_KV_A = 2
N_Q_B = 8
N_KV_B = 2
N_KV_HEADS = N_KV_A + N_KV_B
Q_BLOCK = 128
WINDOW = 128
ROPE_THETA = 10000.0
ATTN_WIDTH = (N_Q_A + N_Q_B) * HEAD_DIM
D_RNN = D_MODEL
RG_BLOCKS = 16
RG_BW = D_RNN // RG_BLOCKS
CONV_W = 4
CONV_LEFT = CONV_W // 2
RG_C = 8.0
PEER_HEADS = 8
N_KEYS = 128
N_EXPERTS = N_KEYS * N_KEYS
PK_DIM = 128
PEER_TOPK = 16
TOK_CHUNK = 128
N_ATTN_LAYERS = (DEPTH + 1) // 2
N_REC_LAYERS = DEPTH // 2
EPS = 1e-6
NEG = -1e30

kernel_name = 'hybrid_dit_prefix_attn_rglru_peer_step'

f32 = jnp.float32


def rmsnorm(x, g):
    xf = x.astype(f32)
    y = xf * lax.rsqrt(jnp.mean(xf * xf, axis=-1, keepdims=True) + EPS) * g.astype(f32)
    return y.astype(x.dtype)


def ada_mod(cvec, w, b):
    m = jax.nn.silu(cvec) @ w + b
    return [t[:, None, :] for t in jnp.split(m, 6, axis=-1)]


def modulate(h, shift, scale):
    return h * (1 + scale) + shift


def axial_rope_tables(n_tok):
    rows = n_tok // GRID_W
    row = jnp.repeat(jnp.arange(rows, dtype=f32), GRID_W)
    col = jnp.tile(jnp.arange(GRID_W, dtype=f32), rows)
    half = HEAD_DIM // 2
    inv = ROPE_THETA ** (-jnp.arange(0, half, 2, dtype=f32) / half)
    ang = jnp.stack([row[:, None] * inv, col[:, None] * inv], axis=1)
    return jnp.cos(ang), jnp.sin(ang)


def apply_rope(x, cos, sin):
    xa = x.astype(f32).reshape(x.shape[:-1] + (2, 2, HEAD_DIM // 4))
    c = cos[None, :, None]
    s = sin[None, :, None]
    x1 = xa[..., 0, :]
    x2 = xa[..., 1, :]
    out = jnp.stack([x1 * c - x2 * s, x2 * c + x1 * s], axis=-2)
    return out.reshape(x.shape).astype(x.dtype)


def attn_project(h, w_in, gqa, gka, gqb, gkb):
    B, T, _ = h.shape
    z = h @ w_in
    sizes = [N_Q_A * HEAD_DIM, N_KV_A * HEAD_DIM, N_KV_A * HEAD_DIM,
             N_Q_B * HEAD_DIM, N_KV_B * HEAD_DIM, N_KV_B * HEAD_DIM]
    qa, ka, va, qb, kb, vb = jnp.split(z, np.cumsum(sizes)[:-1].tolist(), axis=-1)
    qa = rmsnorm(qa.reshape(B, T, N_Q_A, HEAD_DIM), gqa)
    ka = rmsnorm(ka.reshape(B, T, N_KV_A, HEAD_DIM), gka)
    qb = rmsnorm(qb.reshape(B, T, N_Q_B, HEAD_DIM), gqb)
    kb = rmsnorm(kb.reshape(B, T, N_KV_B, HEAD_DIM), gkb)
    k = jnp.concatenate([ka, kb], axis=2)
    v = jnp.concatenate([va.reshape(B, T, N_KV_A, HEAD_DIM), vb.reshape(B, T, N_KV_B, HEAD_DIM)], axis=2)
    return qa, qb, k, v


def dense_gqa(q, k, v, sink):
    B, T, HQ, hd = q.shape
    KV = k.shape[2]
    G = HQ // KV
    nb = T // Q_BLOCK
    qb = q.reshape(B, nb, Q_BLOCK, KV, G, hd).transpose(1, 0, 2, 3, 4, 5)
    scale = HEAD_DIM ** -0.5

    def block(qblk):
        s = jnp.einsum('bqkgd,bskd->bkgqs', qblk, k, preferred_element_type=f32) * scale
        if sink is not None:
            sk = jnp.broadcast_to(sink.astype(f32).reshape(1, KV, G, 1, 1), s.shape[:-1] + (1,))
            p = jax.nn.softmax(jnp.concatenate([s, sk], axis=-1), axis=-1)[..., :-1]
        else:
            p = jax.nn.softmax(s, axis=-1)
        return jnp.einsum('bkgqs,bskd->bqkgd', p.astype(v.dtype), v)

    o = lax.map(block, qb)
    return o.transpose(1, 0, 2, 3, 4, 5).reshape(B, T, HQ * hd)


def banded_gqa(q, k, v, ck, cv, sink):
    B, T, HQ, hd = q.shape
    KV = k.shape[2]
    G = HQ // KV
    nb = T // Q_BLOCK
    L = ck.shape[1]
    scale = HEAD_DIM ** -0.5
    qb = q.reshape(B, nb, Q_BLOCK, KV, G, hd)

    def band(t):
        tb = jnp.pad(t, ((0, 0), (Q_BLOCK, Q_BLOCK), (0, 0), (0, 0))).reshape(B, nb + 2, Q_BLOCK, KV, hd)
        return jnp.concatenate([tb[:, :-2], tb[:, 1:-1], tb[:, 2:]], axis=2)

    kb, vb = band(k), band(v)
    s_band = jnp.einsum('bnqkgd,bnjkd->bnkgqj', qb, kb, preferred_element_type=f32) * scale
    qi = jnp.arange(nb)[:, None, None] * Q_BLOCK + jnp.arange(Q_BLOCK)[None, :, None]
    kj = jnp.arange(nb)[:, None, None] * Q_BLOCK - Q_BLOCK + jnp.arange(3 * Q_BLOCK)[None, None, :]
    valid = (kj >= 0) & (kj < T) & (jnp.abs(qi - kj) <= WINDOW)
    s_band = jnp.where(valid[None, :, None, None], s_band, NEG)
    s_ctx = jnp.einsum('bnqkgd,bckd->bnkgqc', qb, ck, preferred_element_type=f32) * scale
    sk = jnp.broadcast_to(sink.astype(f32).reshape(1, 1, KV, G, 1, 1), s_ctx.shape[:-1] + (1,))
    p = jax.nn.softmax(jnp.concatenate([s_band, s_ctx, sk], axis=-1), axis=-1).astype(v.dtype)
    nband = 3 * Q_BLOCK
    o = (jnp.einsum('bnkgqj,bnjkd->bnqkgd', p[..., :nband], vb)
         + jnp.einsum('bnkgqc,bckd->bnqkgd', p[..., nband:nband + L], cv))
    return o.reshape(B, T, HQ * hd)


def centred_conv(x, w, b):
    y = lax.conv_general_dilated(x, w[:, None, :].astype(x.dtype), window_strides=(1,),
                                 padding=[(CONV_LEFT, CONV_W - 1 - CONV_LEFT)],
                                 dimension_numbers=('NWC', 'WIO', 'NWC'),
                                 feature_group_count=x.shape[-1])
    return y + b


def rg_gates(xc, wa, ba, wx, bx, lam):
    B, T, _ = xc.shape
    xb = xc.reshape(B, T, RG_BLOCKS, RG_BW)
    r = jax.nn.sigmoid(jnp.einsum('btnd,nde->btne', xb, wa.astype(f32)).reshape(B, T, D_RNN) + ba.astype(f32))
    i = jax.nn.sigmoid(jnp.einsum('btnd,nde->btne', xb, wx.astype(f32)).reshape(B, T, D_RNN) + bx.astype(f32))
    log_a = -RG_C * r * jax.nn.softplus(-lam.astype(f32))
    a = jnp.exp(log_a)
    bterm = jnp.sqrt(-jnp.expm1(2.0 * log_a)) * (i * xc)
    return a, bterm


def _lin_combine(e1, e2):
    a1, b1 = e1
    a2, b2 = e2
    return a1 * a2, a2 * b1 + b2


def linear_scan(a, b, h0, reverse):
    first = -1 if reverse else 0
    b = b.at[:, first].add(a[:, first] * h0)
    _, h = lax.associative_scan(_lin_combine, (a, b), reverse=reverse, axis=1)
    return h


def rg_block(h, h0, w_in, conv_w, conv_b, wa, ba, wx, bx, lam, w_out):
    z = h @ w_in
    xr, gate = jnp.split(z, 2, axis=-1)
    xc = centred_conv(xr, conv_w, conv_b).astype(f32)
    a_f, b_f = rg_gates(xc, wa[0], ba[0], wx[0], bx[0], lam[0])
    a_b, b_b = rg_gates(xc, wa[1], ba[1], wx[1], bx[1], lam[1])
    hf = linear_scan(a_f, b_f, h0[:, 0], False)
    hb = linear_scan(a_b, b_b, h0[:, 1], True)
    y = ((hf + hb) * jax.nn.gelu(gate.astype(f32))).astype(h.dtype)
    return y @ w_out, hf, hb


def peer(h, wq, keys, u, v):
    B, T, D = h.shape
    n = B * T
    xt = h.reshape(n, D)
    q = (xt @ wq).reshape(n, PEER_HEADS, 2, PK_DIM)
    s1 = jnp.einsum('nhd,kd->nhk', q[:, :, 0], keys[0], preferred_element_type=f32)
    s2 = jnp.einsum('nhd,kd->nhk', q[:, :, 1], keys[1], preferred_element_type=f32)
    v1, i1 = lax.top_k(s1, PEER_TOPK)
    v2, i2 = lax.top_k(s2, PEER_TOPK)
    cand = (v1[..., :, None] + v2[..., None, :]).reshape(n, PEER_HEADS, PEER_TOPK * PEER_TOPK)
    cidx = (i1[..., :, None] * N_KEYS + i2[..., None, :]).reshape(n, PEER_HEADS, PEER_TOPK * PEER_TOPK)
    top, pos = lax.top_k(cand, PEER_TOPK)
    eidx = jnp.take_along_axis(cidx, pos, axis=-1)
    g = jax.nn.softmax(top, axis=-1).astype(h.dtype)
    nc = n // TOK_CHUNK

    def chunk(args):
        xc, ec, gc = args
        act = jax.nn.gelu(jnp.einsum('cd,chkd->chk', xc, u[ec]))
        return jnp.einsum('chk,chkd->cd', gc * act, v[ec])

    out = lax.map(chunk, (xt.reshape(nc, TOK_CHUNK, D),
                          eidx.reshape(nc, TOK_CHUNK, PEER_HEADS, PEER_TOPK),
                          g.reshape(nc, TOK_CHUNK, PEER_HEADS, PEER_TOPK)))
    return out.reshape(B, T, D)


def setup_inputs(seed: int = 0) -> dict:
    key = jax.random.key(seed)
    ks = iter(jax.random.split(key, 40))
    nrm = lambda shape, s: jax.random.normal(next(ks), shape, f32) * s
    gain = lambda shape: 1.0 + nrm(shape, 0.02)
    u_lam = jax.random.uniform(next(ks), (N_REC_LAYERS, 2, D_RNN), f32, 0.9, 0.999)
    s_lam = u_lam ** (1.0 / RG_C)
    return {
        'x_prompt': nrm((BATCH, SEQ, D_MODEL), 1.0),
        'x_sample': nrm((DEC_BATCH, DEC_SEQ, D_MODEL), 1.0),
        'cache_k': nrm((DEC_BATCH, N_ATTN_LAYERS, PAST_LEN, N_KV_HEADS, HEAD_DIM), 1.0),
        'cache_v': nrm((DEC_BATCH, N_ATTN_LAYERS, PAST_LEN, N_KV_HEADS, HEAD_DIM), 1.0),
        'state_h': nrm((DEC_BATCH, N_REC_LAYERS, 2, D_RNN), 0.5),
        'c': nrm((DEC_BATCH, D_MODEL), 1.0),
        'c_ctx': nrm((D_MODEL,), 1.0),
        'norm1': gain((DEPTH, D_MODEL)),
        'norm2': gain((DEPTH, D_MODEL)),
        'w_mod': nrm((DEPTH, D_MODEL, 6 * D_MODEL), 0.5 * D_MODEL ** -0.5),
        'b_mod': nrm((DEPTH, 6 * D_MODEL), 0.01),
        'w_attn_in': nrm((N_ATTN_LAYERS, D_MODEL, ATTN_WIDTH + 2 * N_KV_HEADS * HEAD_DIM), D_MODEL ** -0.5),
        'w_attn_out': nrm((N_ATTN_LAYERS, ATTN_WIDTH, D_MODEL), ATTN_WIDTH ** -0.5),
        'q_norm_a': gain((N_ATTN_LAYERS, HEAD_DIM)),
        'k_norm_a': gain((N_ATTN_LAYERS, HEAD_DIM)),
        'q_norm_b': gain((N_ATTN_LAYERS, HEAD_DIM)),
        'k_norm_b': gain((N_ATTN_LAYERS, HEAD_DIM)),
        'sink_b': nrm((N_ATTN_LAYERS, N_Q_B), 0.5),
        'w_rg_in': nrm((N_REC_LAYERS, D_MODEL, 2 * D_RNN), D_MODEL ** -0.5),
        'conv_w': nrm((N_REC_LAYERS, CONV_W, D_RNN), CONV_W ** -0.5),
        'conv_b': nrm((N_REC_LAYERS, D_RNN), 0.01),
        'w_rg_a': nrm((N_REC_LAYERS, 2, RG_BLOCKS, RG_BW, RG_BW), RG_BW ** -0.5),
        'b_rg_a': nrm((N_REC_LAYERS, 2, D_RNN), 0.01),
        'w_rg_x': nrm((N_REC_LAYERS, 2, RG_BLOCKS, RG_BW, RG_BW), RG_BW ** -0.5),
        'b_rg_x': nrm((N_REC_LAYERS, 2, D_RNN), 0.01),
        'rg_lambda': jnp.log(s_lam) - jnp.log1p(-s_lam),
        'w_rg_out': nrm((N_REC_LAYERS, D_RNN, D_MODEL), D_RNN ** -0.5),
        'peer_wq': nrm((DEPTH, D_MODEL, PEER_HEADS * 2 * PK_DIM), D_MODEL ** -0.5),
        'peer_keys': nrm((DEPTH, 2, N_KEYS, PK_DIM), PK_DIM ** -0.5),
        'peer_u': nrm((DEPTH, N_EXPERTS, D_MODEL), D_MODEL ** -0.5),
        'peer_v': nrm((DEPTH, N_EXPERTS, D_MODEL), 0.5),
    }


def reference(x_prompt, x_sample, cache_k, cache_v, state_h, c, c_ctx, norm1, norm2, w_mod, b_mod,
              w_attn_in, w_attn_out, q_norm_a, k_norm_a, q_norm_b, k_norm_b, sink_b,
              w_rg_in, conv_w, conv_b, w_rg_a, b_rg_a, w_rg_x, b_rg_x, rg_lambda, w_rg_out,
              peer_wq, peer_keys, peer_u, peer_v):
    xp = x_prompt
    xs = x_sample
    cos, sin = axial_rope_tables(xs.shape[1])
    new_k, new_v, new_h = [], [], []
    for l in range(DEPTH):
        mp = ada_mod(c_ctx[None, :], w_mod[l], b_mod[l])
        ms = ada_mod(c, w_mod[l], b_mod[l])
        hp = modulate(rmsnorm(xp, norm1[l]), mp[0], mp[1])
        hs = modulate(rmsnorm(xs, norm1[l]), ms[0], ms[1])
        j = l // 2
        if l % 2 == 0:
            qa, qb, k, v = attn_project(hp, w_attn_in[j], q_norm_a[j], k_norm_a[j], q_norm_b[j], k_norm_b[j])
            oa = dense_gqa(qa, k[:, :, :N_KV_A], v[:, :, :N_KV_A], None)
            ob = dense_gqa(qb, k[:, :, N_KV_A:], v[:, :, N_KV_A:], sink_b[j])
            yp = jnp.concatenate([oa, ob], axis=-1) @ w_attn_out[j]
            new_k.append(k)
            new_v.append(v)
            qa, qb, kl, vl = attn_project(hs, w_attn_in[j], q_norm_a[j], k_norm_a[j], q_norm_b[j], k_norm_b[j])
            qa = apply_rope(qa, cos, sin)
            qb = apply_rope(qb, cos, sin)
            kl = apply_rope(kl, cos, sin)
            ck = cache_k[:, j]
            cv = cache_v[:, j]
            oa = dense_gqa(qa, jnp.concatenate([ck[:, :, :N_KV_A], kl[:, :, :N_KV_A]], axis=1),
                           jnp.concatenate([cv[:, :, :N_KV_A], vl[:, :, :N_KV_A]], axis=1), None)
            ob = banded_gqa(qb, kl[:, :, N_KV_A:], vl[:, :, N_KV_A:], ck[:, :, N_KV_A:], cv[:, :, N_KV_A:], sink_b[j])
            ys = jnp.concatenate([oa, ob], axis=-1) @ w_attn_out[j]
        else:
            rg_args = (w_rg_in[j], conv_w[j], conv_b[j], w_rg_a[j], b_rg_a[j], w_rg_x[j], b_rg_x[j],
                       rg_lambda[j], w_rg_out[j])
            h0 = jnp.zeros((xp.shape[0], 2, D_RNN), f32)
            yp, hf, hb = rg_block(hp, h0, *rg_args)
            new_h.append(jnp.stack([hf[:, -1], hb[:, 0]], axis=1).astype(xp.dtype))
            ys, _, _ = rg_block(hs, state_h[:, j].astype(f32), *rg_args)
        xp = xp + mp[2] * yp
        xs = xs + ms[2] * ys
        hp = modulate(rmsnorm(xp, norm2[l]), mp[3], mp[4])
        hs = modulate(rmsnorm(xs, norm2[l]), ms[3], ms[4])
        xp = xp + mp[5] * peer(hp, peer_wq[l], peer_keys[l], peer_u[l], peer_v[l])
        xs = xs + ms[5] * peer(hs, peer_wq[l], peer_keys[l], peer_u[l], peer_v[l])
    return (xp, xs, jnp.stack(new_k, axis=1), jnp.stack(new_v, axis=1), jnp.stack(new_h, axis=1))
```

```python
from contextlib import ExitStack
import numpy as np
import concourse.bass as bass
import concourse.mybir as mybir
from concourse.bass_utils import run_bass_kernel_spmd

F32 = mybir.dt.float32
BF16 = mybir.dt.bfloat16
I32 = mybir.dt.int32
U32 = mybir.dt.uint32
AF = mybir.ActivationFunctionType
ALU = mybir.AluOpType

ENGS = ['sync', 'scalar', 'vector', 'gpsimd', 'tensor']
D = 2048
NTOK = 1536
EPS = 1e-6
GELU_K = 1.5957691216057308


class Prog:
    def __init__(self, nc):
        self.nc = nc
        self.ops = {e: [] for e in ENGS}
        self.cnt = {}
        self.last_w = {}
        self.readers = {}
        self.seen = {e: {} for e in ENGS}
        self.sems = {}

    def op(self, eng, fn, reads=(), writes=(), dsem=None):
        need = {}

        def add(t):
            if t is None:
                return
            s, v = t
            if eng == 'tensor' and s == 'E_tensor':
                return
            if v > need.get(s, 0):
                need[s] = v

        for r in reads:
            add(self.last_w.get(r))
        for w in writes:
            add(self.last_w.get(w))
            for s, v in self.readers.get(w, {}).items():
                add((s, v))
        waits = []
        for s, v in need.items():
            if self.seen[eng].get(s, 0) < v:
                waits.append((s, v))
                self.seen[eng][s] = v
        if dsem is None:
            sname, inc = 'E_' + eng, 1
        else:
            sname, inc = 'D_' + dsem, 16
        self.cnt[sname] = self.cnt.get(sname, 0) + inc
        tok = (sname, self.cnt[sname])
        self.ops[eng].append((waits, fn, sname, inc))
        for r in reads:
            self.readers.setdefault(r, {})[sname] = tok[1]
        for w in writes:
            self.last_w[w] = tok
            self.readers[w] = {}
        return tok

    def barrier(self):
        for e in ENGS:
            waits = []
            for s, v in self.cnt.items():
                if e == 'tensor' and s == 'E_tensor':
                    continue
                if self.seen[e].get(s, 0) < v:
                    waits.append((s, v))
                    self.seen[e][s] = v
            if waits:
                self.ops[e].append((waits, None, None, 0))

    def emit(self):
        nc = self.nc
        self.barrier()
        with ExitStack() as st:
            for n in sorted(self.cnt.keys()):
                self.sems[n] = st.enter_context(nc.semaphore(n))
            with nc.Block() as block:
                for eng in ENGS:
                    def body(e, eng=eng):
                        for waits, fn, sname, inc in self.ops[eng]:
                            for (s, v) in waits:
                                e.wait_ge(self.sems[s], v)
                            if fn is not None:
                                fn(e).then_inc(self.sems[sname], inc)
                    getattr(block, eng)(body)


class Arena:
    def __init__(self, t, words):
        self.t, self.W, self.off = t, words, 0

    def take(self, w):
        o = self.off
        self.off += w
        assert self.off <= self.W, (self.off, self.W)
        return o

    def f32(self, n):
        o = self.take(n)
        return self.t[:, o:o + n]

    def bf(self, n):
        w = (n + 1) // 2
        o = self.take(w)
        return self.t[:, o:o + w].bitcast(BF16)

    def i32(self, n):
        o = self.take(n)
        return self.t[:, o:o + n].bitcast(I32)

    def u32(self, n):
        o = self.take(n)
        return self.t[:, o:o + n].bitcast(U32)


class Grp:
    def __init__(self, c, tok0, ntok, seqs):
        self.c, self.tok0, self.ntok, self.seqs = c, tok0, ntok, seqs
        self.nt = ntok // 128
        self.t0 = tok0 // 128


GROUPS = [Grp(0, 0, 512, [(0, 256), (256, 256)]), Grp(1, 512, 1024, [(0, 1024)])]


def build(L=4, peer_last=True, AW=51000):
    nc = bass.Bass("TRN2", target_bir_lowering=False)
    P = Prog(nc)

    def din(name, shape, dt=F32):
        return nc.dram_tensor(name, list(shape), dt, kind="ExternalInput").ap()

    def dout(name, shape, dt=F32):
        return nc.dram_tensor(name, list(shape), dt, kind="ExternalOutput").ap()

    x_in = din("x_in", [NTOK, D])
    ck_d = din("ck", [2, 256, 512])
    cv_d = din("cv", [2, 256, 512])
    h0T_d = din("h0T", [2, 128, 32])
    cT_d = din("cT", [128, 32])
    norm1_d = din("norm1", [4, D])
    norm2_d = din("norm2", [4, D])
    w_mod_d = din("w_mod", [L, D, 6 * D])
    b_mod_d = din("b_mod", [4, 6 * D])
    w_ain_d = din("w_attn_in", [2, D, 3072])
    w_aout_d = din("w_attn_out", [2, D, D])
    qkn_d = din("qkn", [2, 4, 128])
    sink_d = din("sink_b", [2, 8])
    w_rin_d = din("w_rg_in", [2, D, 2 * D])
    rgvec_d = din("rgvec", [2, 128, 11 * 16])
    w_rga_d = din("w_rg_a", [2, 2, 16, 128, 128])
    w_rgx_d = din("w_rg_x", [2, 2, 16, 128, 128])
    w_rout_d = din("w_rg_out", [2, D, D])
    wq_d = din("peer_wq", [4, D, D])
    keys_d = din("peer_keys", [4, 2, 128, 128])
    pu_d = din("peer_u", [L * 16384, D])
    pv_d = din("peer_v", [L * 16384, D])
    cos_d = din("rope_cos", [1024, 64])
    sin_d = din("rope_sin", [1024, 64])

    y_d = dout("y", [NTOK, D])
    nk_d = dout("nk", [2, 2, 256, 512])
    nv_d = dout("nv", [2, 2, 256, 512])
    nh_d = dout("nh", [2, 2, 2, D])

    X_d = nc.dram_tensor("Xs", [NTOK, D], F32, kind="Internal").ap()
    H2_d = nc.dram_tensor("H2s", [NTOK, D], F32, kind="Internal").ap()
    mod_d = nc.dram_tensor("mods", [4, 6, 2, D], F32, kind="Internal").ap()

    with ExitStack() as st:
        arena_t = st.enter_context(nc.sbuf_tensor("arena", [128, AW], F32))
        psb = [st.enter_context(nc.psum_tensor("ps%d" % i, [128, 512], F32))[:, :] for i in range(8)]
        A = Arena(arena_t, AW)

        def dma(eng, out, in_, reads, writes, dsem):
            P.op(eng, lambda e: e.dma_start(out=out, in_=in_), reads, writes, dsem)

        def release(m):
            P.barrier()
            A.off = m

        rot = {}

        def nxt(name, n):
            rot[name] = (rot.get(name, -1) + 1) % n
            return rot[name]

        ident = A.f32(128)
        ones_bf = A.bf(128)
        mprev = A.bf(128)
        mnext = A.bf(128)
        onesf = A.f32(128)
        cos_sb = A.f32(512).rearrange("p (t f) -> p t f", t=8)
        sin_sb = A.f32(512).rearrange("p (t f) -> p t f", t=8)
        junkA = A.bf(2048)
        junkD = A.f32(2048)
        EI = A.i32(12 * 128).rearrange("p (t s) -> p t s", t=12)
        GW = A.f32(12 * 128).rearrange("p (t s) -> p t s", t=12)
        P.op('gpsimd', lambda e: e.memset(ident, 0.0), writes=['ident'])
        P.op('gpsimd', lambda e: e.affine_select(out=ident, in_=ident, pattern=[[-1, 128]], compare_op=ALU.not_equal,
                                                  fill=1.0, base=0, channel_multiplier=1), reads=['ident'], writes=['ident'])
        P.op('gpsimd', lambda e: e.memset(onesf, 1.0), writes=['onesf'])
        P.op('gpsimd', lambda e: e.memset(ones_bf, 1.0), writes=['ones_bf'])
        P.op('gpsimd', lambda e: e.affine_select(out=mprev, in_=ones_bf, pattern=[[-1, 128]], compare_op=ALU.is_ge,
                                                  fill=0.0, base=0, channel_multiplier=1), reads=['ones_bf'], writes=['mprev'])
        P.op('gpsimd', lambda e: e.affine_select(out=mnext, in_=ones_bf, pattern=[[1, 128]], compare_op=ALU.is_ge,
                                                  fill=0.0, base=0, channel_multiplier=-1), reads=['ones_bf'], writes=['mnext'])
        dma('sync', cos_sb, cos_d.rearrange("(t p) f -> p t f", p=128), [], ['cos'], 'cos')
        dma('sync', sin_sb, sin_d.rearrange("(t p) f -> p t f", p=128), [], ['sin'], 'sin')
        base_mark = A.off

        def mod_phase():
            m = A.off
            cT_sb = A.f32(32)
            scT = A.f32(32)
            scT_v = scT.rearrange("p (k c) -> p k c", k=16)
            wbuf = [A.f32(2048) for _ in range(3)]
            mrow = [A.f32(2048) for _ in range(2)]
            brow = [A.f32(2048) for _ in range(2)]
            nrow = [A.f32(2048) for _ in range(2)]
            dma('sync', cT_sb, cT_d, [], ['cT'], 'cT')
            P.op('scalar', lambda e: e.activation(out=scT, in_=cT_sb, func=AF.Silu), reads=['cT'], writes=['scT'])
            i = 0
            gi = 0
            for l in range(L):
                for g in range(6):
                    for kc in range(16):
                        r = i % 3
                        i += 1
                        dma('sync', wbuf[r], w_mod_d[l, kc * 128:(kc + 1) * 128, g * D:(g + 1) * D], [], ['wm%d' % r], 'wm%d' % r)
                        for b in range(4):
                            P.op('tensor', lambda e, b=b, r=r, kc=kc: e.matmul(psb[b][0:2, :], lhsT=scT_v[:, kc, :],
                                 rhs=wbuf[r][:, b * 512:(b + 1) * 512], start=(kc == 0), stop=(kc == 15)),
                                 reads=['wm%d' % r, 'scT'], writes=['ps%d' % b])
                    q = gi % 2
                    gi += 1
                    dma('sync', brow[q][0:2, :], b_mod_d[l:l + 1, g * D:(g + 1) * D].to_broadcast([2, D]), [], ['brow%d' % q], 'brow%d' % q)
                    if g in (1, 4):
                        nd = norm1_d if g == 1 else norm2_d
                        dma('sync', nrow[q][0:2, :], nd[l:l + 1, :].to_broadcast([2, D]), [], ['nrow%d' % q], 'nrow%d' % q)
                    for b in range(4):
                        P.op('vector', lambda e, b=b, q=q: e.tensor_tensor(out=mrow[q][0:2, b * 512:(b + 1) * 512], in0=psb[b][0:2, :],
                             in1=brow[q][0:2, b * 512:(b + 1) * 512], op=ALU.add),
                             reads=['ps%d' % b, 'brow%d' % q], writes=['mrow%d' % q])
                    if g in (1, 4):
                        P.op('vector', lambda e, q=q: e.scalar_tensor_tensor(out=mrow[q][0:2, :], in0=mrow[q][0:2, :], scalar=1.0,
                             in1=nrow[q][0:2, :], op0=ALU.add, op1=ALU.mult), reads=['mrow%d' % q, 'nrow%d' % q], writes=['mrow%d' % q])
                    dma('sync', mod_d[l, g], mrow[q][0:2, :], ['mrow%d' % q], ['mod'], 'st_mrow%d' % q)
            release(m)

        def norm_pass(l, which, grp, src, hT_v, write_H):
            m = A.off
            bcS = A.f32(2048)
            bcB = A.f32(2048)
            xts = [A.f32(2048) for _ in range(2)]
            hxs = [A.f32(2048) for _ in range(2)]
            stt_ = [A.f32(4) for _ in range(2)]
            dma('sync', bcS, mod_d[l, 1 + 3 * which, grp.c:grp.c + 1, :].to_broadcast([128, D]), ['mod'], ['bcS'], 'bcS')
            dma('sync', bcB, mod_d[l, 0 + 3 * which, grp.c:grp.c + 1, :].to_broadcast([128, D]), ['mod'], ['bcB'], 'bcB')
            for tl in range(grp.nt):
                t = grp.t0 + tl
                r = tl % 2
                xt, hx, sv = xts[r], hxs[r], stt_[r]
                dma('sync', xt, src[t * 128:(t + 1) * 128, :], ['X.t%d' % t], ['xt%d' % r], 'xt%d' % r)
                P.op('vector', lambda e, sv=sv: e.memset(sv, 0.0), writes=['nst%d' % r])
                P.op('scalar', lambda e, xt=xt, sv=sv: e.activation(out=junkA, in_=xt, func=AF.Square, accum_out=sv[:, 0:1]),
                     reads=['xt%d' % r, 'nst%d' % r], writes=['junkA', 'nst%d' % r])
                P.op('scalar', lambda e, sv=sv: e.activation(out=sv[:, 1:2], in_=sv[:, 0:1], func=AF.Sqrt, scale=1.0 / D, bias=EPS),
                     reads=['nst%d' % r], writes=['nst%d' % r])
                P.op('vector', lambda e, sv=sv: e.reciprocal(out=sv[:, 2:3], in_=sv[:, 1:2]), reads=['nst%d' % r], writes=['nst%d' % r])
                P.op('vector', lambda e, xt=xt, hx=hx, sv=sv: e.scalar_tensor_tensor(out=hx, in0=xt, scalar=sv[:, 2:3], in1=bcS,
                     op0=ALU.mult, op1=ALU.mult), reads=['xt%d' % r, 'nst%d' % r, 'bcS'], writes=['hx%d' % r])
                P.op('vector', lambda e, hx=hx: e.tensor_tensor(out=hx, in0=hx, in1=bcB, op=ALU.add), reads=['hx%d' % r, 'bcB'], writes=['hx%d' % r])
                if write_H:
                    dma('sync', H2_d[t * 128:(t + 1) * 128, :], hx, ['hx%d' % r], ['H2.t%d' % t], 'st_hx%d' % r)
                for kb in range(4):
                    b = 4 + nxt('tr', 2)
                    for kk in range(4):
                        k = kb * 4 + kk
                        P.op('tensor', lambda e, b=b, kk=kk, k=k, hx=hx: e.transpose(out=psb[b][:, kk * 128:(kk + 1) * 128],
                             in_=hx[:, k * 128:(k + 1) * 128], identity=ident), reads=['hx%d' % r, 'ident'], writes=['ps%d' % b])
                    P.op('scalar', lambda e, b=b, kb=kb, tl=tl: e.activation(out=hT_v[:, kb * 4:kb * 4 + 4, tl * 128:(tl + 1) * 128],
                         in_=psb[b].rearrange("p (a f) -> p a f", a=4), func=AF.Copy), reads=['ps%d' % b], writes=['hT.%d' % tl])
            return m

        def load_wb(W2d, KC, c0, ncols, wst, wb_v, r):
            per = max(1, 2048 // ncols)
            for k0 in range(0, KC, per):
                s = nxt('wst', 2)
                n = min(per, KC - k0)
                sv = wst[s][:, 0:n * ncols].rearrange("p (k n) -> p k n", k=n)
                dma('sync', sv, W2d.rearrange("(k p) n -> p k n", p=128)[:, k0:k0 + n, c0:c0 + ncols], [], ['wst%d' % s], 'wst%d' % s)
                P.op('gpsimd', lambda e, sv=sv, k0=k0, n=n: e.tensor_copy(out=wb_v[:, k0:k0 + n, 0:ncols], in_=sv),
                     reads=['wst%d' % s], writes=['wb%d' % r])

        def proj_tok(W2d, KC, AT_v, akey, ncols, ntiles, consumer, wst, wbs):
            for cg in range(ncols // 512):
                r = nxt('wb', 2)
                wb_v = wbs[r]
                load_wb(W2d, KC, cg * 512, 512, wst, wb_v, r)
                for tl in range(ntiles):
                    b = nxt('mm', 4)
                    for kc in range(KC):
                        P.op('tensor', lambda e, b=b, kc=kc, tl=tl, wb_v=wb_v: e.matmul(psb[b], lhsT=AT_v[:, kc, tl * 128:(tl + 1) * 128],
                             rhs=wb_v[:, kc, :], start=(kc == 0), stop=(kc == KC - 1)),
                             reads=['wb%d' % r, akey(tl)], writes=['ps%d' % b])
                    consumer(cg, tl, b)

        def resid_consumer(l, grp, src, dst, gate_idx, xps, tmps, gbc):
            def cons(cg, tl, b):
                t = grp.t0 + tl
                r = nxt('xp', 2)
                xp, tmp = xps[r], tmps[r]
                rows = slice(t * 128, (t + 1) * 128)
                cols = slice(cg * 512, (cg + 1) * 512)
                dma('sync', xp, src[rows, cols], ['X.t%d' % t], ['xp%d' % r], 'xp%d' % r)
                P.op('vector', lambda e: e.tensor_tensor(out=tmp, in0=psb[b], in1=gbc[:, cols], op=ALU.mult),
                     reads=['ps%d' % b, 'gbc'], writes=['tmp%d' % r])
                P.op('gpsimd', lambda e: e.tensor_tensor(out=xp, in0=tmp, in1=xp, op=ALU.add), reads=['tmp%d' % r, 'xp%d' % r], writes=['xp%d' % r])
                dkey = 'X.t%d' % t
                dma('sync', dst[rows, cols], xp, ['xp%d' % r], [dkey], 'st_xp%d' % r)
            return cons

        def gelu_tanh(eng_v, out, x, tmp, rk, wk, n):
            P.op('vector', lambda e: e.tensor_tensor(out=tmp, in0=x, in1=x, op=ALU.mult), reads=rk, writes=wk + ['gl_tmp%s' % n])
            P.op('vector', lambda e: e.tensor_scalar(out=tmp, in0=tmp, scalar1=0.044715, scalar2=1.0, op0=ALU.mult, op1=ALU.add),
                 reads=['gl_tmp%s' % n], writes=['gl_tmp%s' % n])
            P.op('vector', lambda e: e.tensor_tensor(out=tmp, in0=tmp, in1=x, op=ALU.mult), reads=rk + ['gl_tmp%s' % n], writes=['gl_tmp%s' % n])
            P.op('scalar', lambda e: e.activation(out=tmp, in_=tmp, func=AF.Sigmoid, scale=GELU_K), reads=['gl_tmp%s' % n], writes=['gl_tmp%s' % n])
            P.op('vector', lambda e: e.tensor_tensor(out=out, in0=tmp, in1=x, op=ALU.mult), reads=rk + ['gl_tmp%s' % n], writes=wk)

        def attn_group(l, grp, src, dst):
            j = l // 2
            is_s = grp.c == 1
            nt = grp.nt
            nkt = nt + (2 if is_s else 0)
            koff = 256 if is_s else 0
            m0 = A.off
            QT = A.bf(16 * grp.ntok).rearrange("p (h t) -> p h t", h=16)
            KT = A.bf(4 * nkt * 128).rearrange("p (h t) -> p h t", h=4)
            V = A.bf(nkt * 512).rearrange("p (t h d) -> p t h d", t=nkt, h=4)
            gnb = A.f32(512).rearrange("p (a d) -> p a d", a=4)
            esink = A.f32(8)
            dma('sync', gnb, qkn_d[j:j + 1].to_broadcast([128, 4, 128]), [], ['gnb'], 'gnb')
            dma('sync', esink, sink_d[j:j + 1, :].to_broadcast([128, 8]), [], ['esink'], 'esink')
            P.op('scalar', lambda e: e.activation(out=esink, in_=esink, func=AF.Exp), reads=['esink'], writes=['esink'])
            m1 = A.off
            hT = A.bf(16 * grp.ntok).rearrange("p (k t) -> p k t", k=16)
            mN = A.off
            norm_pass(l, 0, grp, src, hT, False)
            release(mN)
            wst = [A.f32(2048) for _ in range(2)]
            wbs = [A.bf(16 * 512).rearrange("p (k n) -> p k n", k=16) for _ in range(2)]
            qn = [A.f32(512).rearrange("p (a d) -> p a d", a=4) for _ in range(2)]
            qr = [A.f32(512).rearrange("p (a d) -> p a d", a=4) for _ in range(2)]
            rt = [A.f32(512) for _ in range(2)]
            vst = [A.f32(256) for _ in range(2)]
            sq = [A.f32(16) for _ in range(2)]
            if is_s:
                cst = [A.f32(512) for _ in range(2)]
                for c in range(2):
                    dma('sync', cst[0], ck_d[j, c * 128:(c + 1) * 128, :], [], ['cst0'], 'cst0')
                    dma('sync', cst[1], cv_d[j, c * 128:(c + 1) * 128, :], [], ['cst1'], 'cst1')
                    b = 4 + nxt('tr', 2)
                    for h in range(4):
                        P.op('tensor', lambda e, b=b, h=h: e.transpose(out=psb[b][:, h * 128:(h + 1) * 128], in_=cst[0][:, h * 128:(h + 1) * 128],
                             identity=ident), reads=['cst0', 'ident'], writes=['ps%d' % b])
                    P.op('scalar', lambda e, b=b, c=c: e.activation(out=KT[:, :, c * 128:(c + 1) * 128], in_=psb[b].rearrange("p (a f) -> p a f", a=4),
                         func=AF.Copy), reads=['ps%d' % b], writes=['KT'])
                    P.op('vector', lambda e, c=c: e.tensor_copy(out=V[:, c, :, :], in_=cst[1].rearrange("p (h d) -> p h d", h=4)),
                         reads=['cst1'], writes=['V'])

            def qkv_cons(cg, tl, b):
                t = grp.t0 + tl
                r = nxt('qk', 2)
                pz = psb[b]
                is_q = cg in (0, 1, 3, 4)
                nh = 4 if is_q else 2
                gi = {0: 0, 1: 0, 2: 1, 3: 2, 4: 2, 5: 3}[cg]
                s_ = sq[r]
                P.op('vector', lambda e: e.memset(s_, 0.0), writes=['sq%d' % r])
                for i in range(nh):
                    P.op('scalar', lambda e, i=i: e.activation(out=junkA[:, 0:128], in_=pz[:, i * 128:(i + 1) * 128], func=AF.Square,
                         accum_out=s_[:, i:i + 1]), reads=['ps%d' % b, 'sq%d' % r], writes=['junkA', 'sq%d' % r])
                P.op('scalar', lambda e: e.activation(out=s_[:, 4:4 + nh], in_=s_[:, 0:nh], func=AF.Sqrt, scale=1.0 / 128, bias=EPS),
                     reads=['sq%d' % r], writes=['sq%d' % r])
                P.op('vector', lambda e: e.reciprocal(out=s_[:, 8:8 + nh], in_=s_[:, 4:4 + nh]), reads=['sq%d' % r], writes=['sq%d' % r])
                for i in range(nh):
                    P.op('vector', lambda e, i=i: e.scalar_tensor_tensor(out=qn[r][:, i, :], in0=pz[:, i * 128:(i + 1) * 128], scalar=s_[:, 8 + i:9 + i],
                         in1=gnb[:, gi, :], op0=ALU.mult, op1=ALU.mult), reads=['ps%d' % b, 'sq%d' % r, 'gnb'], writes=['qn%d' % r])
                srcq = qn[r]
                skey = 'qn%d' % r
                if is_s:
                    xv = qn[r][:, 0:nh, :].rearrange("p h (a s f) -> p h a s f", a=2, s=2)
                    ov = qr[r][:, 0:nh, :].rearrange("p h (a s f) -> p h a s f", a=2, s=2)
                    tv = rt[r][:, 0:nh * 64].rearrange("p (h a f) -> p h a f", h=nh, a=2)
                    cb = cos_sb[:, tl, :].rearrange("p (a f) -> p a f", a=2).unsqueeze(1).to_broadcast([128, nh, 2, 32])
                    sb_ = sin_sb[:, tl, :].rearrange("p (a f) -> p a f", a=2).unsqueeze(1).to_broadcast([128, nh, 2, 32])
                    x1, x2 = xv[:, :, :, 0, :], xv[:, :, :, 1, :]
                    o1, o2 = ov[:, :, :, 0, :], ov[:, :, :, 1, :]
                    rk = ['qn%d' % r, 'cos', 'sin']
                    P.op('vector', lambda e: e.tensor_tensor(out=o1, in0=x1, in1=cb, op=ALU.mult), reads=rk, writes=['qr%d' % r])
                    P.op('vector', lambda e: e.tensor_tensor(out=tv, in0=x2, in1=sb_, op=ALU.mult), reads=rk, writes=['rt%d' % r])
                    P.op('vector', lambda e: e.tensor_tensor(out=o1, in0=o1, in1=tv, op=ALU.subtract), reads=['qr%d' % r, 'rt%d' % r], writes=['qr%d' % r])
                    P.op('vector', lambda e: e.tensor_tensor(out=o2, in0=x2, in1=cb, op=ALU.mult), reads=rk, writes=['qr%d' % r])
                    P.op('vector', lambda e: e.tensor_tensor(out=tv, in0=x1, in1=sb_, op=ALU.mult), reads=rk + ['rt%d' % r], writes=['rt%d' % r])
                    P.op('vector', lambda e: e.tensor_tensor(out=o2, in0=o2, in1=tv, op=ALU.add), reads=['qr%d' % r, 'rt%d' % r], writes=['qr%d' % r])
                    srcq = qr[r]
                    skey = 'qr%d' % r
                if (not is_q) and (not is_s):
                    sidx, toff = divmod(tl * 128, 256)
                    kv0 = 0 if cg == 2 else 256
                    dma('sync', nk_d[grp_seq0 + sidx, j, toff:toff + 128, kv0:kv0 + 256], qn[r][:, 0:2, :].rearrange("p a d -> p (a d)"), ['qn%d' % r], [], 'st_qn%d' % r)
                bt = 4 + nxt('tr', 2)
                for i in range(nh):
                    P.op('tensor', lambda e, i=i: e.transpose(out=psb[bt][:, i * 128:(i + 1) * 128], in_=srcq[:, i, :], identity=ident),
                         reads=[skey, 'ident'], writes=['ps%d' % bt])
                pv = psb[bt][:, 0:nh * 128].rearrange("p (a f) -> p a f", a=nh)
                if is_q:
                    hq0 = {0: 0, 1: 4, 3: 8, 4: 12}[cg]
                    P.op('scalar', lambda e: e.activation(out=QT[:, hq0:hq0 + 4, tl * 128:(tl + 1) * 128], in_=pv, func=AF.Copy),
                         reads=['ps%d' % bt], writes=['QT'])
                else:
                    kv0 = 0 if cg == 2 else 2
                    P.op('scalar', lambda e: e.activation(out=KT[:, kv0:kv0 + 2, koff + tl * 128:koff + (tl + 1) * 128], in_=pv, func=AF.Copy),
                         reads=['ps%d' % bt], writes=['KT'])
                    vt = tl + (2 if is_s else 0)
                    P.op('scalar', lambda e: e.activation(out=V[:, vt, kv0:kv0 + 2, :], in_=pz[:, 256:512].rearrange("p (h d) -> p h d", h=2),
                         func=AF.Copy), reads=['ps%d' % b], writes=['V'])
                    if not is_s:
                        sidx, toff = divmod(tl * 128, 256)
                        P.op('vector', lambda e: e.tensor_copy(out=vst[r], in_=pz[:, 256:512]), reads=['ps%d' % b], writes=['vst%d' % r])
                        dma('sync', nv_d[grp_seq0 + sidx, j, toff:toff + 128, kv0 * 128:kv0 * 128 + 256], vst[r], ['vst%d' % r], [], 'st_vst%d' % r)

            grp_seq0 = 0
            proj_tok(w_ain_d[j], 16, hT, lambda tl: 'hT.%d' % tl, 3072, nt, qkv_cons, wst, wbs)
            release(m1)
            OT = A.bf(16 * grp.ntok).rearrange("p (h t) -> p h t", h=16)
            PT = [A.bf(512) for _ in range(3)]
            rden = [A.f32(512) for _ in range(2)]
            scale = 128 ** -0.5

            def attn_block(hq, kvh, q0, nq, keyt, sink):
                po, pd = psb[6], psb[7]
                n = len(keyt)
                for ii, (kt, mk) in enumerate(keyt):
                    bs = 4 + nxt('tr', 2)
                    pr = nxt('PT', 3)
                    P.op('tensor', lambda e, bs=bs, kt=kt: e.matmul(psb[bs][:, 0:nq], lhsT=KT[:, kvh, kt * 128:(kt + 1) * 128], rhs=QT[:, hq, q0:q0 + nq],
                         start=True, stop=True), reads=['KT', 'QT'], writes=['ps%d' % bs])
                    P.op('scalar', lambda e, bs=bs, pr=pr: e.activation(out=PT[pr][:, 0:nq], in_=psb[bs][:, 0:nq], func=AF.Exp, scale=scale),
                         reads=['ps%d' % bs], writes=['PT%d' % pr])
                    if mk is not None:
                        P.op('gpsimd', lambda e, pr=pr, mk=mk: e.tensor_tensor(out=PT[pr][:, 0:nq], in0=PT[pr][:, 0:nq], in1=mk, op=ALU.mult),
                             reads=['PT%d' % pr, 'mprev', 'mnext'], writes=['PT%d' % pr])
                    P.op('tensor', lambda e, pr=pr, kt=kt, ii=ii: e.matmul(po[:, 0:nq], lhsT=V[:, kt, kvh, :], rhs=PT[pr][:, 0:nq], start=(ii == 0), stop=(ii == n - 1)),
                         reads=['V', 'PT%d' % pr], writes=['ps6'])
                    P.op('tensor', lambda e, pr=pr, ii=ii: e.matmul(pd[:, 0:nq], lhsT=ones_bf, rhs=PT[pr][:, 0:nq], start=(ii == 0), stop=(ii == n - 1)),
                         reads=['ones_bf', 'PT%d' % pr], writes=['ps7'])
                rr = nxt('rden', 2)
                if sink is not None:
                    P.op('vector', lambda e: e.tensor_scalar(out=rden[rr][:, 0:nq], in0=pd[:, 0:nq], scalar1=esink[:, sink:sink + 1], scalar2=None, op0=ALU.add),
                         reads=['ps7', 'esink'], writes=['rden%d' % rr])
                    P.op('vector', lambda e: e.reciprocal(out=rden[rr][:, 0:nq], in_=rden[rr][:, 0:nq]), reads=['rden%d' % rr], writes=['rden%d' % rr])
                else:
                    P.op('vector', lambda e: e.reciprocal(out=rden[rr][:, 0:nq], in_=pd[:, 0:nq]), reads=['ps7'], writes=['rden%d' % rr])
                P.op('vector', lambda e: e.tensor_tensor(out=OT[:, hq, q0:q0 + nq], in0=po[:, 0:nq], in1=rden[rr][:, 0:nq], op=ALU.mult),
                     reads=['ps6', 'rden%d' % rr], writes=['OT'])

            for hq in range(16):
                isB = hq >= 8
                kvh = (hq // 4) if not isB else 2 + (hq - 8) // 4
                sink = (hq - 8) if isB else None
                if not is_s:
                    for s in range(2):
                        attn_block(hq, kvh, s * 256, 256, [(2 * s, None), (2 * s + 1, None)], sink)
                elif not isB:
                    for qh in range(2):
                        attn_block(hq, kvh, qh * 512, 512, [(kt, None) for kt in range(10)], None)
                else:
                    for qt in range(8):
                        keyt = [(0, None), (1, None)]
                        if qt > 0:
                            keyt.append((2 + qt - 1, mprev))
                        keyt.append((2 + qt, None))
                        if qt < 7:
                            keyt.append((2 + qt + 1, mnext))
                        attn_block(hq, kvh, qt * 128, 128, keyt, sink)
            m2 = A.off
            wst = [A.f32(2048) for _ in range(2)]
            wbs = [A.bf(16 * 512).rearrange("p (k n) -> p k n", k=16) for _ in range(2)]
            xps = [A.f32(512) for _ in range(2)]
            tmps = [A.f32(512) for _ in range(2)]
            gbc = A.f32(2048)
            dma('sync', gbc, mod_d[l, 2, grp.c:grp.c + 1, :].to_broadcast([128, D]), ['mod'], ['gbc'], 'gbc')
            proj_tok(w_aout_d[j], 16, OT, lambda tl: 'OT', 2048, nt, resid_consumer(l, grp, src, dst, 2, xps, tmps, gbc), wst, wbs)
            release(m0)

        def rg_group(l, grp, src, dst):
            j = l // 2
            is_s = grp.c == 1
            nt = grp.nt
            m0 = A.off
            YT = A.bf(16 * grp.ntok).rearrange("p (k t) -> p k t", k=16)
            m1 = A.off
            hT = A.bf(16 * grp.ntok).rearrange("p (k t) -> p k t", k=16)
            mN = A.off
            norm_pass(l, 0, grp, src, hT, False)
            release(mN)
            Ds = []
            pos = 0
            for (s0, ln) in grp.seqs:
                Ds.append(pos + 2)
                pos += ln + 3
            W = pos
            rv = A.f32(176).rearrange("p (v k) -> p v k", v=11)
            c8 = A.f32(64).rearrange("p (a d k) -> p a d k", a=2, d=2)
            h0v = A.f32(32).rearrange("p (d k) -> p d k", d=2)
            nhs = A.f32(64)
            nhT = A.f32(128)
            dma('sync', rv, rgvec_d[j].rearrange("p (v k) -> p v k", v=11), [], ['rv'], 'rv')
            dma('sync', h0v, h0T_d[j].rearrange("p (d k) -> p d k", d=2), [], ['h0v'], 'h0v')
            P.op('scalar', lambda e: e.activation(out=c8[:, 0, :, :], in_=rv[:, 9:11, :], func=AF.Exp, scale=-1.0), reads=['rv'], writes=['c8'])
            P.op('scalar', lambda e: e.activation(out=c8[:, 0, :, :], in_=c8[:, 0, :, :], func=AF.Ln, bias=1.0), reads=['c8'], writes=['c8'])
            P.op('vector', lambda e: e.tensor_scalar(out=c8[:, 1, :, :], in0=c8[:, 0, :, :], scalar1=-16.0, scalar2=None, op0=ALU.mult), reads=['c8'], writes=['c8'])
            P.op('vector', lambda e: e.tensor_scalar(out=c8[:, 0, :, :], in0=c8[:, 0, :, :], scalar1=-8.0, scalar2=None, op0=ALU.mult), reads=['c8'], writes=['c8'])
            wstx = [A.f32(2048) for _ in range(2)]
            wbx = [A.bf(2048).rearrange("p (k n) -> p k n", k=16) for _ in range(2)]
            wbg = [A.bf(2048).rearrange("p (k n) -> p k n", k=16) for _ in range(2)]
            wg = [A.f32(512).rearrange("p (a n) -> p a n", a=4) for _ in range(2)]
            xrp = A.f32(W)
            xc = A.f32(W)
            gt = A.f32(W)
            gl = A.f32(W)
            gtmp = A.f32(W)
            rb = [A.f32(W) for _ in range(2)]
            ib = [A.f32(W) for _ in range(2)]
            ab = [A.f32(W) for _ in range(2)]
            bb = [A.f32(W) for _ in range(2)]
            hh = [A.f32(W) for _ in range(2)]
            P.op('vector', lambda e: e.memset(xrp, 0.0), writes=['xrp'])
            for bufs_, nm in ((rb, 'rb'), (ib, 'ib')):
                for d in range(2):
                    P.op('gpsimd', lambda e, bb_=bufs_[d]: e.memset(bb_, 0.0), writes=['%s%d' % (nm, d)])
            P.op('gpsimd', lambda e: e.memset(gt, 0.0), writes=['gt'])
            ranges = []
            if is_s:
                ranges = [(0, 512, [(0, 512, 2)]), (512, 512, [(0, 512, 514)])]
                dranges = [(2, 512), (514, 512)]
            else:
                ranges = [(0, 512, [(0, 256, Ds[0]), (256, 256, Ds[1])])]
                dranges = [(Ds[0], 256), (Ds[1], 256)]
            for c in range(16):
                r = c % 2
                load_wb(w_rin_d[j], 16, c * 128, 128, wstx, wbx[r], r)
                per = 16
                s = nxt('wst', 2)
                sv = wstx[s].rearrange("p (k n) -> p k n", k=16)
                dma('sync', sv, w_rin_d[j].rearrange("(k p) n -> p k n", p=128)[:, :, D + c * 128:D + (c + 1) * 128], [], ['wst%d' % s], 'wst%d' % s)
                P.op('gpsimd', lambda e, sv=sv, r=r: e.tensor_copy(out=wbg[r], in_=sv), reads=['wst%d' % s], writes=['wbg%d' % r])
                for a_, wd in enumerate((w_rga_d, w_rgx_d)):
                    for d in range(2):
                        dma('sync', wg[r][:, a_ * 2 + d, :], wd[j, d, c], [], ['wg%d' % r], 'wg%d' % r)
                for (tk0, n, pieces) in ranges:
                    b = nxt('mm', 4)
                    for kc in range(16):
                        P.op('tensor', lambda e, b=b, kc=kc, tk0=tk0, n=n, r=r: e.matmul(psb[b][:, 0:n], lhsT=wbx[r][:, kc, :], rhs=hT[:, kc, tk0:tk0 + n],
                             start=(kc == 0), stop=(kc == 15)), reads=['wb%d' % r] + ['hT.%d' % tl for tl in range(nt)], writes=['ps%d' % b])
                    for (c0, ln, dp) in pieces:
                        P.op('scalar', lambda e, b=b, c0=c0, ln=ln, dp=dp: e.activation(out=xrp[:, dp:dp + ln], in_=psb[b][:, c0:c0 + ln], func=AF.Copy),
                             reads=['ps%d' % b], writes=['xrp'])
                    b2 = nxt('mm', 4)
                    for kc in range(16):
                        P.op('tensor', lambda e, b2=b2, kc=kc, tk0=tk0, n=n, r=r: e.matmul(psb[b2][:, 0:n], lhsT=wbg[r][:, kc, :], rhs=hT[:, kc, tk0:tk0 + n],
                             start=(kc == 0), stop=(kc == 15)), reads=['wbg%d' % r] + ['hT.%d' % tl for tl in range(nt)], writes=['ps%d' % b2])
                    for (c0, ln, dp) in pieces:
                        P.op('scalar', lambda e, b2=b2, c0=c0, ln=ln, dp=dp: e.activation(out=gt[:, dp:dp + ln], in_=psb[b2][:, c0:c0 + ln], func=AF.Copy),
                             reads=['ps%d' % b2], writes=['gt'])
                n_ = W - 3
                P.op('vector', lambda e, c=c: e.tensor_scalar(out=xc[:, 2:2 + n_], in0=xrp[:, 0:n_], scalar1=rv[:, 0, c:c + 1], scalar2=rv[:, 4, c:c + 1],
                     op0=ALU.mult, op1=ALU.add), reads=['xrp', 'rv'], writes=['xc'])
                for k in range(1, 4):
                    P.op('vector', lambda e, c=c, k=k: e.scalar_tensor_tensor(out=xc[:, 2:2 + n_], in0=xrp[:, k:k + n_], scalar=rv[:, k, c:c + 1],
                         in1=xc[:, 2:2 + n_], op0=ALU.mult, op1=ALU.add), reads=['xrp', 'rv', 'xc'], writes=['xc'])
                gelu_tanh('vector', gl, gt, gtmp, ['gt'], ['gl'], 'rg')
                for d in range(2):
                    for (dp, ln) in dranges:
                        b = nxt('mm', 4)
                        P.op('tensor', lambda e, b=b, d=d, dp=dp, ln=ln, r=r: e.matmul(psb[b][:, 0:ln], lhsT=wg[r][:, d, :], rhs=xc[:, dp:dp + ln], start=True, stop=True),
                             reads=['wg%d' % r, 'xc'], writes=['ps%d' % b])
                        P.op('scalar', lambda e, b=b, d=d, dp=dp, ln=ln, c=c: e.activation(out=rb[d][:, dp:dp + ln], in_=psb[b][:, 0:ln], func=AF.Sigmoid,
                             bias=rv[:, 5 + d, c:c + 1], scale=1.0), reads=['ps%d' % b, 'rv'], writes=['rb%d' % d])
                        b = nxt('mm', 4)
                        P.op('tensor', lambda e, b=b, d=d, dp=dp, ln=ln, r=r: e.matmul(psb[b][:, 0:ln], lhsT=wg[r][:, 2 + d, :], rhs=xc[:, dp:dp + ln], start=True, stop=True),
                             reads=['wg%d' % r, 'xc'], writes=['ps%d' % b])
                        P.op('scalar', lambda e, b=b, d=d, dp=dp, ln=ln, c=c: e.activation(out=ib[d][:, dp:dp + ln], in_=psb[b][:, 0:ln], func=AF.Sigmoid,
                             bias=rv[:, 7 + d, c:c + 1], scale=1.0), reads=['ps%d' % b, 'rv'], writes=['ib%d' % d])
                    P.op('scalar', lambda e, d=d, c=c: e.activation(out=ab[d], in_=rb[d], func=AF.Exp, scale=c8[:, 0, d, c:c + 1]), reads=['rb%d' % d, 'c8'], writes=['ab%d' % d])
                    P.op('scalar', lambda e, d=d, c=c: e.activation(out=bb[d], in_=rb[d], func=AF.Exp, scale=c8[:, 1, d, c:c + 1]), reads=['rb%d' % d, 'c8'], writes=['bb%d' % d])
                    P.op('scalar', lambda e, d=d: e.activation(out=bb[d], in_=bb[d], func=AF.Sqrt, scale=-1.0, bias=1.0), reads=['bb%d' % d], writes=['bb%d' % d])
                    P.op('vector', lambda e, d=d: e.tensor_tensor(out=bb[d], in0=bb[d], in1=ib[d], op=ALU.mult), reads=['bb%d' % d, 'ib%d' % d], writes=['bb%d' % d])
                    P.op('vector', lambda e, d=d: e.tensor_tensor(out=bb[d][:, 2:2 + n_], in0=bb[d][:, 2:2 + n_], in1=xc[:, 2:2 + n_], op=ALU.mult),
                         reads=['bb%d' % d, 'xc'], writes=['bb%d' % d])
                    for si, (s0, ln) in enumerate(grp.seqs):
                        dp = Ds[si]
                        init = h0v[:, d, c:c + 1] if is_s else 0.0
                        if d == 0:
                            P.op('vector', lambda e, dp=dp, ln=ln, init=init: e.tensor_tensor_scan(out=hh[0][:, dp:dp + ln], data0=ab[0][:, dp:dp + ln],
                                 data1=bb[0][:, dp:dp + ln], initial=init, op0=ALU.mult, op1=ALU.add), reads=['ab0', 'bb0', 'h0v'], writes=['hh0'])
                        else:
                            P.op('vector', lambda e, dp=dp, ln=ln, init=init: e.tensor_tensor_scan(out=hh[1][:, dp:dp + ln][:, ::-1], data0=ab[1][:, dp:dp + ln][:, ::-1],
                                 data1=bb[1][:, dp:dp + ln][:, ::-1], initial=init, op0=ALU.mult, op1=ALU.add), reads=['ab1', 'bb1', 'h0v'], writes=['hh1'])
                        if not is_s:
                            col = (si * 2 + d) * 16 + c
                            pos_ = dp + ln - 1 if d == 0 else dp
                            P.op('vector', lambda e, col=col, pos_=pos_, d=d: e.tensor_copy(out=nhs[:, col:col + 1], in_=hh[d][:, pos_:pos_ + 1]),
                                 reads=['hh%d' % d], writes=['nhs'])
                P.op('vector', lambda e: e.tensor_tensor(out=hh[0], in0=hh[0], in1=hh[1], op=ALU.add), reads=['hh0', 'hh1'], writes=['hh0'])
                for si, (s0, ln) in enumerate(grp.seqs):
                    dp = Ds[si]
                    P.op('vector', lambda e, dp=dp, ln=ln, s0=s0, c=c: e.tensor_tensor(out=YT[:, c, s0:s0 + ln], in0=hh[0][:, dp:dp + ln], in1=gl[:, dp:dp + ln], op=ALU.mult),
                         reads=['hh0', 'gl'], writes=['YT'])
            if not is_s:
                P.op('tensor', lambda e: e.transpose(out=psb[4][0:64, 0:128], in_=nhs, identity=ident), reads=['nhs', 'ident'], writes=['ps4'])
                P.op('vector', lambda e: e.tensor_copy(out=nhT[0:64, :], in_=psb[4][0:64, 0:128]), reads=['ps4'], writes=['nhT'])
                for s in range(2):
                    dma('sync', nh_d[s, j].rearrange("d (c p) -> (d c) p", p=128), nhT[s * 32:(s + 1) * 32, :], ['nhT'], [], 'st_nhT')
            release(m1)
            wst = [A.f32(2048) for _ in range(2)]
            wbs = [A.bf(16 * 512).rearrange("p (k n) -> p k n", k=16) for _ in range(2)]
            xps = [A.f32(512) for _ in range(2)]
            tmps = [A.f32(512) for _ in range(2)]
            gbc = A.f32(2048)
            dma('sync', gbc, mod_d[l, 2, grp.c:grp.c + 1, :].to_broadcast([128, D]), ['mod'], ['gbc'], 'gbc')
            proj_tok(w_rout_d[j], 16, YT, lambda tl: 'YT', 2048, nt, resid_consumer(l, grp, src, dst, 2, xps, tmps, gbc), wst, wbs)
            release(m0)

        def peer_route(l, grp, src):
            m0 = A.off
            nt = grp.nt
            PQ = A.bf(16 * grp.ntok).rearrange("p (h t) -> p h t", h=16)
            keysT = A.bf(256).rearrange("p (a k) -> p a k", a=2)
            kst = A.f32(256).rearrange("p (a d) -> p a d", a=2)
            m1 = A.off
            hT = A.bf(16 * grp.ntok).rearrange("p (k t) -> p k t", k=16)
            norm_pass(l, 1, grp, src, hT, True)
            wst = [A.f32(2048) for _ in range(2)]
            wbs = [A.bf(16 * 512).rearrange("p (k n) -> p k n", k=16) for _ in range(2)]
            dma('sync', kst, keys_d[l].rearrange("a k d -> k a d"), [], ['kst'], 'kst')
            for a_ in range(2):
                P.op('tensor', lambda e, a_=a_: e.transpose(out=psb[4][:, a_ * 128:(a_ + 1) * 128], in_=kst[:, a_, :], identity=ident),
                     reads=['kst', 'ident'], writes=['ps4'])
            P.op('scalar', lambda e: e.activation(out=keysT, in_=psb[4][:, 0:256].rearrange("p (a k) -> p a k", a=2), func=AF.Copy), reads=['ps4'], writes=['keysT'])
            hkeys = ['hT.%d' % tl for tl in range(nt)]
            for cg in range(4):
                r = nxt('wb', 2)
                load_wb(wq_d[l], 16, cg * 512, 512, wst, wbs[r], r)
                for cc in range(4):
                    jj = cg * 4 + cc
                    for q0 in range(0, grp.ntok, 512):
                        b = nxt('mm', 4)
                        for kc in range(16):
                            P.op('tensor', lambda e, b=b, kc=kc, cc=cc, q0=q0, r=r: e.matmul(psb[b], lhsT=wbs[r][:, kc, cc * 128:(cc + 1) * 128], rhs=hT[:, kc, q0:q0 + 512],
                                 start=(kc == 0), stop=(kc == 15)), reads=['wb%d' % r] + hkeys, writes=['ps%d' % b])
                        P.op('scalar', lambda e, b=b, jj=jj, q0=q0: e.activation(out=PQ[:, jj, q0:q0 + 512], in_=psb[b], func=AF.Copy), reads=['ps%d' % b], writes=['PQ'])
            release(m1)
            SC = A.f32(2048).rearrange("p (j k) -> p j k", j=16)
            SC2 = A.f32(128)
            mx = A.f32(256).rearrange("p (h a k) -> p h a k", h=8, a=2)
            ix = A.u32(256).rearrange("p (h a k) -> p h a k", h=8, a=2)
            ixf = A.f32(256).rearrange("p (h a k) -> p h a k", h=8, a=2)
            cand = A.f32(2048).rearrange("p (h a b) -> p h a b", h=8, a=16)
            cand2 = A.f32(256)
            cidx = A.f32(2048).rearrange("p (h a b) -> p h a b", h=8, a=16)
            top = A.f32(128).rearrange("p (h k) -> p h k", h=8)
            ef = A.f32(128).rearrange("p (h k) -> p h k", h=8)
            eg = A.f32(128).rearrange("p (h k) -> p h k", h=8)
            sm = A.f32(24)
            for tl in range(nt):
                t = grp.t0 + tl
                for q4 in range(4):
                    b = nxt('mm', 4)
                    for cc in range(4):
                        jj = q4 * 4 + cc
                        P.op('tensor', lambda e, b=b, cc=cc, jj=jj, tl=tl: e.matmul(psb[b][:, cc * 128:(cc + 1) * 128], lhsT=PQ[:, jj, tl * 128:(tl + 1) * 128],
                             rhs=keysT[:, jj % 2, :], start=True, stop=True), reads=['PQ', 'keysT'], writes=['ps%d' % b])
                    P.op('scalar', lambda e, b=b, q4=q4: e.activation(out=SC[:, q4 * 4:q4 * 4 + 4, :], in_=psb[b].rearrange("p (a f) -> p a f", a=4), func=AF.Copy),
                         reads=['ps%d' % b], writes=['SC'])
                for jj in range(16):
                    h, a_ = jj // 2, jj % 2
                    P.op('vector', lambda e, jj=jj, h=h, a_=a_: e.max(out=mx[:, h, a_, 0:8], in_=SC[:, jj, :]), reads=['SC'], writes=['mx'])
                    P.op('vector', lambda e, jj=jj, h=h, a_=a_: e.max_index(out=ix[:, h, a_, 0:8], in_max=mx[:, h, a_, 0:8], in_values=SC[:, jj, :]),
                         reads=['SC', 'mx'], writes=['ix'])
                    P.op('vector', lambda e, jj=jj, h=h, a_=a_: e.match_replace(out=SC2, in_to_replace=mx[:, h, a_, 0:8], in_values=SC[:, jj, :], imm_value=-1e30),
                         reads=['SC', 'mx'], writes=['SC2'])
                    P.op('vector', lambda e, h=h, a_=a_: e.max(out=mx[:, h, a_, 8:16], in_=SC2), reads=['SC2'], writes=['mx'])
                    P.op('vector', lambda e, h=h, a_=a_: e.max_index(out=ix[:, h, a_, 8:16], in_max=mx[:, h, a_, 8:16], in_values=SC2),
                         reads=['SC2', 'mx'], writes=['ix'])
                P.op('vector', lambda e: e.tensor_copy(out=ixf, in_=ix), reads=['ix'], writes=['ixf'])
                P.op('vector', lambda e: e.memset(ef, 0.0), writes=['ef'])
                P.op('vector', lambda e: e.memset(sm, 0.0), writes=['sm'])
                for h in range(8):
                    v1 = mx[:, h, 0, :].unsqueeze(2).to_broadcast([128, 16, 16])
                    v2 = mx[:, h, 1, :].unsqueeze(1).to_broadcast([128, 16, 16])
                    i1 = ixf[:, h, 0, :].unsqueeze(2).to_broadcast([128, 16, 16])
                    i2 = ixf[:, h, 1, :].unsqueeze(1).to_broadcast([128, 16, 16])
                    P.op('vector', lambda e, h=h, v1=v1, v2=v2: e.tensor_tensor(out=cand[:, h, :, :], in0=v1, in1=v2, op=ALU.add), reads=['mx'], writes=['cand'])
                    P.op('vector', lambda e, h=h, i1=i1, i2=i2: e.scalar_tensor_tensor(out=cidx[:, h, :, :], in0=i1, scalar=128.0, in1=i2, op0=ALU.mult, op1=ALU.add),
                         reads=['ixf'], writes=['cidx'])
                    cf = cand[:, h, :, :].rearrange("p a b -> p (a b)")
                    xf = cidx[:, h, :, :].rearrange("p a b -> p (a b)")
                    P.op('vector', lambda e, h=h, cf=cf: e.max(out=top[:, h, 0:8], in_=cf), reads=['cand'], writes=['top'])
                    P.op('vector', lambda e, h=h, cf=cf: e.match_replace(out=cand2, in_to_replace=top[:, h, 0:8], in_values=cf, imm_value=-1e30),
                         reads=['cand', 'top'], writes=['cand2'])
                    P.op('vector', lambda e, h=h: e.max(out=top[:, h, 8:16], in_=cand2), reads=['cand2'], writes=['top'])
                    for k in range(16):
                        P.op('vector', lambda e, h=h, k=k, cf=cf, xf=xf: e.scalar_tensor_tensor(out=junkD[:, 0:256], in0=cf, scalar=top[:, h, k:k + 1], in1=xf,
                             op0=ALU.is_equal, op1=ALU.mult, accum_out=ef[:, h, k:k + 1]), reads=['cand', 'cidx', 'top', 'ef'], writes=['junkD', 'ef'])
                    P.op('vector', lambda e, h=h: e.tensor_scalar(out=sm[:, h:h + 1], in0=top[:, h, 0:1], scalar1=-1.0, scalar2=None, op0=ALU.mult), reads=['top', 'sm'], writes=['sm'])
                    P.op('scalar', lambda e, h=h: e.activation(out=eg[:, h, :], in_=top[:, h, :], func=AF.Exp, bias=sm[:, h:h + 1], scale=1.0, accum_out=sm[:, 8 + h:9 + h]),
                         reads=['top', 'sm'], writes=['eg', 'sm'])
                P.op('vector', lambda e: e.reciprocal(out=sm[:, 16:24], in_=sm[:, 8:16]), reads=['sm'], writes=['sm'])
                P.op('vector', lambda e, t=t: e.tensor_tensor(out=GW[:, t, :].rearrange("p (h k) -> p h k", h=8), in0=eg, in1=sm[:, 16:24].unsqueeze(2).to_broadcast([128, 8, 16]),
                     op=ALU.mult), reads=['eg', 'sm'], writes=['GW.%d' % t])
                P.op('vector', lambda e: e.tensor_scalar(out=ef, in0=ef, scalar1=16383.0, scalar2=float(l * 16384), op0=ALU.min, op1=ALU.add), reads=['ef'], writes=['ef'])
                P.op('vector', lambda e, t=t: e.tensor_copy(out=EI[:, t, :], in_=ef.rearrange("p h k -> p (h k)")), reads=['ef'], writes=['EI.%d' % t])
            release(m0)

        def peer_apply(l, src, dst, NB=6):
            m0 = A.off
            gb = [A.f32(2048) for _ in range(NB)]
            h2 = [A.f32(2048) for _ in range(2)]
            xt = [A.f32(2048) for _ in range(2)]
            acc = A.f32(2048)
            g2 = [A.f32(2048) for _ in range(2)]
            Aact = A.f32(128)
            Wt = A.f32(128)
            gtmp = A.f32(128)
            for c in range(2):
                dma('sync', g2[c], mod_d[l, 5, c:c + 1, :].to_broadcast([128, D]), ['mod'], ['g2%d' % c], 'g2%d' % c)
            for t in range(12):
                c = 0 if t < 4 else 1
                r = t % 2
                rows = slice(t * 128, (t + 1) * 128)
                dma('sync', h2[r], H2_d[rows, :], ['H2.t%d' % t], ['h2%d' % r], 'h2%d' % r)
                dma('sync', xt[r], src[rows, :], ['X.t%d' % t], ['pxt%d' % r], 'pxt%d' % r)
                P.op('vector', lambda e: e.memset(Aact, 0.0), writes=['Aact'])
                for s in range(128):
                    q = nxt('gb', NB)
                    P.op('gpsimd', lambda e, q=q, s=s, t=t: e.indirect_dma_start(out=gb[q], out_offset=None, in_=pu_d,
                         in_offset=bass.IndirectOffsetOnAxis(ap=EI[:, t, s:s + 1], axis=0)), reads=['EI.%d' % t], writes=['gb%d' % q], dsem='gb%d' % q)
                    P.op('vector', lambda e, q=q, s=s, r=r: e.scalar_tensor_tensor(out=junkD, in0=gb[q], scalar=1.0, in1=h2[r], op0=ALU.mult, op1=ALU.mult,
                         accum_out=Aact[:, s:s + 1]), reads=['gb%d' % q, 'h2%d' % r, 'Aact'], writes=['junkD', 'Aact'])
                gelu_tanh('vector', Wt, Aact, gtmp, ['Aact'], ['Wt'], 'pe')
                P.op('vector', lambda e, t=t: e.tensor_tensor(out=Wt, in0=Wt, in1=GW[:, t, :], op=ALU.mult), reads=['Wt', 'GW.%d' % t], writes=['Wt'])
                for s in range(128):
                    q = nxt('gb', NB)
                    P.op('gpsimd', lambda e, q=q, s=s, t=t: e.indirect_dma_start(out=gb[q], out_offset=None, in_=pv_d,
                         in_offset=bass.IndirectOffsetOnAxis(ap=EI[:, t, s:s + 1], axis=0)), reads=['EI.%d' % t], writes=['gb%d' % q], dsem='gb%d' % q)
                    if s == 0:
                        P.op('vector', lambda e, q=q: e.tensor_scalar(out=acc, in0=gb[q], scalar1=Wt[:, 0:1], scalar2=None, op0=ALU.mult),
                             reads=['gb%d' % q, 'Wt'], writes=['acc'])
                    else:
                        P.op('vector', lambda e, q=q, s=s: e.scalar_tensor_tensor(out=acc, in0=gb[q], scalar=Wt[:, s:s + 1], in1=acc, op0=ALU.mult, op1=ALU.add),
                             reads=['gb%d' % q, 'Wt', 'acc'], writes=['acc'])
                P.op('vector', lambda e, c=c: e.tensor_tensor(out=acc, in0=acc, in1=g2[c], op=ALU.mult), reads=['acc', 'g2%d' % c], writes=['acc'])
                P.op('vector', lambda e, r=r: e.tensor_tensor(out=xt[r], in0=acc, in1=xt[r], op=ALU.add), reads=['acc', 'pxt%d' % r], writes=['pxt%d' % r])
                dma('sync', dst[rows, :], xt[r], ['pxt%d' % r], ['X.t%d' % t], 'st_pxt%d' % r)
            release(m0)

        mod_phase()
        for l in range(L):
            src = x_in if l == 0 else X_d
            last = (l == L - 1)
            do_peer = peer_last or not last
            dst = X_d if do_peer else y_d
            for grp in GROUPS:
                if l % 2 == 0:
                    attn_group(l, grp, src, dst)
                else:
                    rg_group(l, grp, src, dst)
            if do_peer:
                for grp in GROUPS:
                    peer_route(l, grp, X_d)
                peer_apply(l, X_d, y_d if last else X_d)
        P.emit()
    return nc


def _rope_tables():
    rows = 1024 // 64
    row = np.repeat(np.arange(rows, dtype=np.float32), 64)
    col = np.tile(np.arange(64, dtype=np.float32), rows)
    inv = (np.float32(10000.0) ** (-np.arange(0, 64, 2, dtype=np.float32) / np.float32(64))).astype(np.float32)
    ang = np.stack([row[:, None] * inv, col[:, None] * inv], axis=1).astype(np.float32)
    return np.cos(ang).reshape(1024, 64).astype(np.float32), np.sin(ang).reshape(1024, 64).astype(np.float32)


def _fm(v):
    return np.ascontiguousarray(np.asarray(v).reshape(16, 128).T)


def make_in_maps(inp, cores, L=4):
    f = lambda a: np.ascontiguousarray(np.asarray(a, dtype=np.float32))
    cos, sin = _rope_tables()
    shared = dict(
        norm1=f(inp['norm1']), norm2=f(inp['norm2']), w_mod=f(np.asarray(inp['w_mod'])[:L]), b_mod=f(inp['b_mod']),
        w_attn_in=f(inp['w_attn_in']), w_attn_out=f(inp['w_attn_out']),
        qkn=f(np.stack([inp['q_norm_a'], inp['k_norm_a'], inp['q_norm_b'], inp['k_norm_b']], axis=1)),
        sink_b=f(inp['sink_b']), w_rg_in=f(inp['w_rg_in']), w_rg_a=f(inp['w_rg_a']), w_rg_x=f(inp['w_rg_x']),
        w_rg_out=f(inp['w_rg_out']), peer_wq=f(inp['peer_wq']), peer_keys=f(inp['peer_keys']),
        peer_u=f(np.asarray(inp['peer_u'])[:L]).reshape(L * 16384, D), peer_v=f(np.asarray(inp['peer_v'])[:L]).reshape(L * 16384, D), rope_cos=cos, rope_sin=sin)
    rgvec = np.zeros((2, 128, 11, 16), np.float32)
    for j in range(2):
        vecs = [inp['conv_w'][j][k] for k in range(4)] + [inp['conv_b'][j], inp['b_rg_a'][j][0], inp['b_rg_a'][j][1],
                                                           inp['b_rg_x'][j][0], inp['b_rg_x'][j][1], inp['rg_lambda'][j][0], inp['rg_lambda'][j][1]]
        for vi, v in enumerate(vecs):
            rgvec[j, :, vi, :] = _fm(v)
    shared['rgvec'] = rgvec.reshape(2, 128, 176)
    maps = []
    for i in cores:
        b = i // 4
        xp = np.asarray(inp['x_prompt'])[2 * i:2 * i + 2].reshape(512, D)
        xs = np.asarray(inp['x_sample'])[b]
        m = dict(shared)
        m['x_in'] = f(np.concatenate([xp, xs], axis=0))
        m['ck'] = f(np.asarray(inp['cache_k'])[b].reshape(2, 256, 512))
        m['cv'] = f(np.asarray(inp['cache_v'])[b].reshape(2, 256, 512))
        h0 = np.asarray(inp['state_h'])[b]
        m['h0T'] = f(np.stack([np.stack([_fm(h0[j, d]) for d in range(2)], axis=1) for j in range(2)], axis=0).reshape(2, 128, 32))
        cT = np.stack([_fm(inp['c_ctx']), _fm(np.asarray(inp['c'])[b])], axis=2)
        m['cT'] = f(cT.reshape(128, 32))
        maps.append(m)
    return maps


_NC_CACHE = {}


def kernel(**inputs):
    if 'nc' not in _NC_CACHE:
        _NC_CACHE['nc'] = build()
    nc = _NC_CACHE['nc']
    maps = make_in_maps(inputs, list(range(8)))
    res = run_bass_kernel_spmd(nc, maps, core_ids=list(range(8)))
    R = res.results
    y_prompt = np.concatenate([R[i]['y'][:512].reshape(2, 256, D) for i in range(8)], axis=0)
    y_sample = np.stack([R[0]['y'][512:], R[4]['y'][512:]], axis=0)
    nk = np.concatenate([R[i]['nk'].reshape(2, 2, 256, 4, 128) for i in range(8)], axis=0)
    nv = np.concatenate([R[i]['nv'].reshape(2, 2, 256, 4, 128) for i in range(8)], axis=0)
    nh = np.concatenate([R[i]['nh'] for i in range(8)], axis=0)
    return (y_prompt.astype(np.float32), y_sample.astype(np.float32), nk.astype(np.float32), nv.astype(np.float32), nh.astype(np.float32))
```
